# Optimizing a Trainium2 kernel written in Bass

```python
import math
import jax, jax.numpy as jnp
from jax import lax
import numpy as np

D_MODEL = 1024
BATCH = 8
SEQ = 4096
DEPTH = 4

N_MEM = 256
NORM_EPS = 1e-6
A_HEAD_DIM = 64
A_WIDTH = D_MODEL // 2
A_HEADS = A_WIDTH // A_HEAD_DIM
A_DECAY_LORA = 64
A_ICLR_LORA = 64
A_GATE_LORA = 128
A_COLS = 3 * A_WIDTH + A_DECAY_LORA + A_ICLR_LORA + A_GATE_LORA
A_GN_EPS = 64e-5
B_HEAD_DIM = 64
B_WIDTH = D_MODEL // 2
B_HEADS = B_WIDTH // B_HEAD_DIM
B_COLS = 4 * B_WIDTH
RET_CHUNK = 128
ROPE_BASE = 10000.0
HYB_COLS = A_COLS + B_COLS
C_HEAD_DIM = 64
C_V_DIM = 2 * C_HEAD_DIM
C_HEADS = D_MODEL // C_V_DIM
Q_BLOCK = 128
REL_BUCKETS = 32
REL_MAX_DIST = 128
X_HEADS = 4
X_HEAD_DIM = D_MODEL // X_HEADS
D_FF = 4 * D_MODEL
N_EVEN = (DEPTH + 1) // 2
N_ODD = DEPTH // 2

kernel_name = "hybrid_rwkv7_retnet_diffattn_trunk"


def rms_norm(x, w, eps=NORM_EPS):
    xf = x.astype(jnp.float32)
    y = xf * lax.rsqrt(jnp.mean(xf * xf, axis=-1, keepdims=True) + eps)
    return y.astype(x.dtype) * w


def group_norm(x, w, b, eps):
    xf = x.astype(jnp.float32)
    mu = jnp.mean(xf, axis=-1, keepdims=True)
    var = jnp.mean(jnp.square(xf - mu), axis=-1, keepdims=True)
    y = ((xf - mu) * lax.rsqrt(var + eps)).astype(x.dtype)
    h, d = x.shape[-2:]
    return y * w.reshape(h, d) + b.reshape(h, d)


def token_shift(x):
    return jnp.pad(x[:, :-1], ((0, 0), (1, 0), (0, 0)))


def rotary(x):
    s, d = x.shape[1], x.shape[-1]
    inv = ROPE_BASE ** (-jnp.arange(0, d, 2, dtype=jnp.float32) / d)
    ang = jnp.arange(s, dtype=jnp.float32)[:, None] * inv[None, :]
    cos = jnp.cos(ang)[None, :, None, :]
    sin = jnp.sin(ang)[None, :, None, :]
    x1, x2 = jnp.split(x, 2, axis=-1)
    return jnp.concatenate([x1 * cos - x2 * sin, x1 * sin + x2 * cos], axis=-1).astype(x.dtype)


def t5_bucket(rel):
    n = jnp.maximum(rel, 0)
    max_exact = REL_BUCKETS // 2
    large = max_exact + (jnp.log(jnp.maximum(n, max_exact).astype(jnp.float32) / max_exact)
                         / math.log(REL_MAX_DIST / max_exact) * (REL_BUCKETS - max_exact)).astype(jnp.int32)
    large = jnp.minimum(large, REL_BUCKETS - 1)
    return jnp.where(n < max_exact, n, large)


def wkv7_scan(r, w, k, v, a, b):
    dt = r.dtype
    bsz, _, h, n = r.shape
    xs = tuple(jnp.moveaxis(t.astype(jnp.float32), 1, 0) for t in (r, w, k, v, a, b))

    def step(state, inp):
        r_t, w_t, k_t, v_t, a_t, b_t = inp
        sa = jnp.einsum('bhvk,bhk->bhv', state, a_t)
        state = (state * w_t[:, :, None, :] + sa[..., None] * b_t[:, :, None, :]
                 + v_t[..., None] * k_t[:, :, None, :])
        return state, jnp.einsum('bhvk,bhk->bhv', state, r_t)

    s0 = jnp.zeros((bsz, h, n, n), jnp.float32)
    _, ys = lax.scan(step, s0, xs)
    return jnp.moveaxis(ys, 0, 1).astype(dt)


def rwkv7_time_mix(pa, mu, w0, w2, a0, a2, g2, k_k, k_a, r_k, ln_w, ln_b):
    bsz, s, _ = pa.shape
    pa = pa + (token_shift(pa) - pa) * mu
    c1 = A_WIDTH
    c3 = 3 * A_WIDTH
    r, k, v, wd, ad, gd = jnp.split(pa, [c1, 2 * c1, c3, c3 + A_DECAY_LORA, c3 + A_DECAY_LORA + A_ICLR_LORA], axis=-1)
    w_log = -jax.nn.softplus(-(w0 + jnp.tanh(wd) @ w2)) - 0.5
    decay = jnp.exp(-jnp.exp(w_log.astype(jnp.float32)))
    a = jax.nn.sigmoid(a0 + ad @ a2)
    g = jax.nn.sigmoid(gd) @ g2
    hd = lambda t: t.reshape(bsz, s, A_HEADS, A_HEAD_DIM)
    kk = hd(k * k_k).astype(jnp.float32)
    kk = (kk / jnp.maximum(jnp.sqrt(jnp.sum(kk * kk, axis=-1, keepdims=True)), 1e-12)).astype(pa.dtype)
    k = k * (1.0 + (a - 1.0) * k_a)
    r_h, k_h, v_h, a_h = hd(r), hd(k), hd(v), hd(a)
    y = wkv7_scan(r_h, hd(decay), k_h, v_h, -kk, kk * a_h)
    y = group_norm(y, ln_w, ln_b, A_GN_EPS)
    y = y + jnp.sum(r_h * k_h * r_k, axis=-1, keepdims=True) * v_h
    return y.reshape(bsz, s, A_WIDTH) * g


def retention(pb, ln_w):
    bsz, s, _ = pb.shape
    q, k, v, g = jnp.split(pb, 4, axis=-1)
    q = rotary(q.reshape(bsz, s, B_HEADS, B_HEAD_DIM))
    k = rotary(k.reshape(bsz, s, B_HEADS, B_HEAD_DIM)) * (B_HEAD_DIM ** -0.5)
    v = v.reshape(bsz, s, B_HEADS, B_HEAD_DIM)
    n_chunks = s // RET_CHUNK
    to_chunks = lambda t: t.reshape(bsz, n_chunks, RET_CHUNK, B_HEADS, -1).transpose(0, 3, 1, 2, 4).astype(jnp.float32)
    qc, kc, vc = to_chunks(q), to_chunks(k), to_chunks(v)
    gamma = 1.0 - 2.0 ** (-5.0 - jnp.arange(B_HEADS, dtype=jnp.float32))
    lg = jnp.log(gamma)[:, None]
    idx = jnp.arange(RET_CHUNK, dtype=jnp.float32)
    rel = idx[:, None] - idx[None, :]
    dmat = jnp.where(rel >= 0, jnp.exp(jnp.maximum(rel, 0.0)[None] * lg[..., None]), 0.0)
    scores = jnp.einsum('bhncd,bhnmd->bhncm', qc, kc) * dmat[None, :, None]
    inner = jnp.einsum('bhncm,bhnme->bhnce', scores, vc)
    zeta = jnp.exp((RET_CHUNK - 1 - idx)[None, :] * lg)
    xi = jnp.exp((idx + 1)[None, :] * lg)
    chunk_decay = jnp.exp(RET_CHUNK * lg)[:, :, None]
    upd = jnp.einsum('bhnmd,bhnme->bhnde', kc * zeta[None, :, None, :, None], vc)

    def step(state, u_i):
        return state * chunk_decay[None] + u_i, state

    s0 = jnp.zeros((bsz, B_HEADS, B_HEAD_DIM, B_HEAD_DIM), jnp.float32)
    _, r_prev = lax.scan(step, s0, jnp.moveaxis(upd, 2, 0))
    r_prev = jnp.moveaxis(r_prev, 0, 2)
    cross = jnp.einsum('bhncd,bhnde->bhnce', qc * xi[None, :, None, :, None], r_prev)
    o = (inner + cross).transpose(0, 2, 3, 1, 4).reshape(bsz, s, B_HEADS, B_HEAD_DIM).astype(pb.dtype)
    o = rms_norm(o, ln_w.reshape(B_HEADS, B_HEAD_DIM))
    return o.reshape(bsz, s, B_WIDTH) * jax.nn.silu(g)


def diff_attention(h, w_in, lq1, lk1, lq2, lk2, ln_w, w_out, rel_bias, lam_init):
    bsz, s, _ = h.shape
    q, k, v = jnp.split(h @ w_in, 3, axis=-1)
    q = q.reshape(bsz, s, C_HEADS, 2, C_HEAD_DIM).transpose(0, 2, 3, 1, 4)
    k = k.reshape(bsz, s, C_HEADS, 2, C_HEAD_DIM).transpose(0, 2, 3, 1, 4)
    v = v.reshape(bsz, s, C_HEADS, C_V_DIM).transpose(0, 2, 1, 3)
    lam = (jnp.exp(jnp.sum(lq1 * lk1).astype(jnp.float32))
           - jnp.exp(jnp.sum(lq2 * lk2).astype(jnp.float32)) + lam_init)
    n_blk = s // Q_BLOCK
    qb = q.reshape(bsz, C_HEADS, 2, n_blk, Q_BLOCK, C_HEAD_DIM).transpose(3, 0, 1, 2, 4, 5)
    k_pos = jnp.arange(s, dtype=jnp.int32)
    scale = C_HEAD_DIM ** -0.5

    def block(args):
        q_blk, blk = args
        q_pos = blk * Q_BLOCK + jnp.arange(Q_BLOCK, dtype=jnp.int32)
        rel = q_pos[:, None] - k_pos[None, :]
        bias = rel_bias[t5_bucket(rel)].transpose(2, 0, 1).astype(jnp.float32)
        logits = jnp.einsum('bhcqd,bhckd->bhcqk', q_blk, k).astype(jnp.float32) * scale + bias[None, :, None]
        logits = jnp.where(rel >= 0, logits, -jnp.inf)
        p = jax.nn.softmax(logits, axis=-1)
        attn = p[:, :, 0] - lam * p[:, :, 1]
        return jnp.einsum('bhqk,bhke->bhqe', attn.astype(v.dtype), v)

    o = lax.map(block, (qb, jnp.arange(n_blk, dtype=jnp.int32)))
    o = o.transpose(1, 0, 3, 2, 4).reshape(bsz, s, C_HEADS, C_V_DIM)
    o = rms_norm(o, ln_w) * (1.0 - lam_init)
    return o.reshape(bsz, s, C_HEADS * C_V_DIM) @ w_out


def memory_cross_attention(h, mem_n, w_q, w_kv, w_o):
    bsz, s, _ = h.shape
    m = mem_n.shape[1]
    q = (h @ w_q).reshape(bsz, s, X_HEADS, X_HEAD_DIM)
    k, v = jnp.split(mem_n @ w_kv, 2, axis=-1)
    k = k.reshape(bsz, m, X_HEADS, X_HEAD_DIM)
    v = v.reshape(bsz, m, X_HEADS, X_HEAD_DIM)
    logits = jnp.einsum('bshd,bmhd->bhsm', q, k).astype(jnp.float32) * (X_HEAD_DIM ** -0.5)
    p = jax.nn.softmax(logits, axis=-1)
    o = jnp.einsum('bhsm,bmhd->bshd', p.astype(v.dtype), v)
    return o.reshape(bsz, s, D_MODEL) @ w_o


def squared_relu_mlp(h, w1, w2):
    return jnp.square(jax.nn.relu(h @ w1)) @ w2


def setup_inputs(seed: int = 0) -> dict:
    key = jax.random.key(seed)
    ks = iter(jax.random.split(key, 48))
    f32 = jnp.float32
    D = D_MODEL

    def nrm(shape, scale):
        return jax.random.normal(next(ks), shape, f32) * scale

    def gain(shape):
        return 1.0 + nrm(shape, 0.02)

    return {
        "x": nrm((BATCH, SEQ, D), 1.0),
        "mem": nrm((BATCH, N_MEM, D), 1.0),
        "rel_bias": nrm((REL_BUCKETS, C_HEADS), 0.5),
        "mem_norm_w": gain((D,)),
        "final_norm_w": gain((D,)),
        "norm_mix_w": gain((DEPTH, D)),
        "norm_cross_w": gain((DEPTH, D)),
        "norm_mlp_w": gain((DEPTH, D)),
        "xattn_w_q": nrm((DEPTH, D, D), D ** -0.5),
        "xattn_w_kv": nrm((DEPTH, D, 2 * D), D ** -0.5),
        "xattn_w_o": nrm((DEPTH, D, D), D ** -0.5),
        "mlp_w1": nrm((DEPTH, D, D_FF), D ** -0.5),
        "mlp_w2": nrm((DEPTH, D_FF, D), D_FF ** -0.5),
        "hyb_w_in": nrm((N_EVEN, D, HYB_COLS), D ** -0.5),
        "rwkv_mu": jax.random.uniform(next(ks), (N_EVEN, A_COLS), f32),
        "rwkv_w0": jax.random.uniform(next(ks), (N_EVEN, A_WIDTH), f32, minval=-6.5, maxval=-1.0),
        "rwkv_w2": nrm((N_EVEN, A_DECAY_LORA, A_WIDTH), 0.5 * A_DECAY_LORA ** -0.5),
        "rwkv_a0": nrm((N_EVEN, A_WIDTH), 0.5),
        "rwkv_a2": nrm((N_EVEN, A_ICLR_LORA, A_WIDTH), A_ICLR_LORA ** -0.5),
        "rwkv_g2": nrm((N_EVEN, A_GATE_LORA, A_WIDTH), A_GATE_LORA ** -0.5),
        "rwkv_k_k": 0.85 + nrm((N_EVEN, A_WIDTH), 0.05),
        "rwkv_k_a": 1.0 + nrm((N_EVEN, A_WIDTH), 0.05),
        "rwkv_r_k": nrm((N_EVEN, A_HEADS, A_HEAD_DIM), 0.1),
        "rwkv_ln_w": gain((N_EVEN, A_WIDTH)),
        "rwkv_ln_b": nrm((N_EVEN, A_WIDTH), 0.02),
        "ret_ln_w": gain((N_EVEN, B_WIDTH)),
        "hyb_w_out": nrm((N_EVEN, A_WIDTH + B_WIDTH, D), (A_WIDTH + B_WIDTH) ** -0.5),
        "diff_w_in": nrm((N_ODD, D, 3 * D), D ** -0.5),
        "diff_lq1": nrm((N_ODD, C_HEAD_DIM), 0.1),
        "diff_lk1": nrm((N_ODD, C_HEAD_DIM), 0.1),
        "diff_lq2": nrm((N_ODD, C_HEAD_DIM), 0.1),
        "diff_lk2": nrm((N_ODD, C_HEAD_DIM), 0.1),
        "diff_ln_w": gain((N_ODD, C_V_DIM)),
        "diff_w_out": nrm((N_ODD, C_HEADS * C_V_DIM, D), D ** -0.5),
    }


def reference(x, mem, rel_bias, mem_norm_w, final_norm_w, norm_mix_w, norm_cross_w, norm_mlp_w,
              xattn_w_q, xattn_w_kv, xattn_w_o, mlp_w1, mlp_w2, hyb_w_in, rwkv_mu, rwkv_w0, rwkv_w2,
              rwkv_a0, rwkv_a2, rwkv_g2, rwkv_k_k, rwkv_k_a, rwkv_r_k, rwkv_ln_w, rwkv_ln_b, ret_ln_w,
              hyb_w_out, diff_w_in, diff_lq1, diff_lk1, diff_lq2, diff_lk2, diff_ln_w, diff_w_out):
    mem_n = rms_norm(mem, mem_norm_w)
    h = x
    for layer in range(DEPTH):
        i = layer // 2
        hn = rms_norm(h, norm_mix_w[layer])
        if layer % 2 == 0:
            proj = hn @ hyb_w_in[i]
            y_a = rwkv7_time_mix(proj[..., :A_COLS], rwkv_mu[i], rwkv_w0[i], rwkv_w2[i], rwkv_a0[i],
                                 rwkv_a2[i], rwkv_g2[i], rwkv_k_k[i], rwkv_k_a[i], rwkv_r_k[i],
                                 rwkv_ln_w[i], rwkv_ln_b[i])
            y_b = retention(proj[..., A_COLS:], ret_ln_w[i])
            mix = jnp.concatenate([y_a, y_b], axis=-1) @ hyb_w_out[i]
        else:
            lam_init = 0.8 - 0.6 * math.exp(-0.3 * layer)
            mix = diff_attention(hn, diff_w_in[i], diff_lq1[i], diff_lk1[i], diff_lq2[i], diff_lk2[i],
                                 diff_ln_w[i], diff_w_out[i], rel_bias, lam_init)
        h = h + mix
        h = h + memory_cross_attention(rms_norm(h, norm_cross_w[layer]), mem_n,
                                       xattn_w_q[layer], xattn_w_kv[layer], xattn_w_o[layer])
        h = h + squared_relu_mlp(rms_norm(h, norm_mlp_w[layer]), mlp_w1[layer], mlp_w2[layer])
    return rms_norm(h, final_norm_w)
```

```python
import math
from contextlib import ExitStack
import numpy as np
import concourse.bass as bass
import concourse.mybir as mybir
from concourse.bass_utils import run_bass_kernel_spmd

F32 = mybir.dt.float32
BF16 = mybir.dt.bfloat16
AF = mybir.ActivationFunctionType
ALU = mybir.AluOpType
AX = mybir.AxisListType

D = 1024
KC = 8
NMEM = 256
DEPTH = 4
A_COLS = 1792
HYB_COLS = 3840
EPS = 1e-6
GN_EPS = 64e-5
NEG = -30000.0
import os
CUT = int(os.environ.get('CUT', '99'))


class Slot:
    __slots__ = ("w", "r", "rd")

    def __init__(self):
        self.w = None
        self.r = {}
        self.rd = []


class Op:
    __slots__ = ("eng", "fn", "deps", "dma", "ndma", "sig", "sigidx", "dcount", "idx")


def slots(n):
    return [Slot() for _ in range(n)]


class Prog:
    SELF_SYNC = ("act", "dve", "pool")

    def __init__(self):
        self.ops = {e: [] for e in ("pe", "act", "dve", "pool", "sp")}
        self.n = 0
        self.dma_keys = {}
        self.pending = {}
        self.dma_since = []

    def barrier(self):
        last = set(self.dma_since)
        for e, lst in self.ops.items():
            for o in reversed(lst):
                if o.dma is None:
                    last.add(o)
                    break
        for e in self.ops:
            self.pending[e] = set(last) | self.pending.get(e, set())
        self.dma_since = []

    def add(self, eng, fn, reads=(), writes=(), dma=None, ndma=1):
        o = Op()
        o.eng = eng; o.fn = fn; o.dma = dma; o.ndma = ndma; o.sig = False; o.sigidx = 0
        o.idx = self.n; self.n += 1
        deps = set()
        for s in reads:
            if s.w is not None:
                deps.add(s.w)
        for s in writes:
            if s.w is not None:
                deps.add(s.w)
            deps.update(s.r.values())
            deps.update(s.rd)
        for s in reads:
            if dma is None:
                s.r[eng] = o
            else:
                s.rd.append(o)
        for s in writes:
            s.w = o; s.r = {}; s.rd = []
        if eng in self.pending:
            deps |= self.pending.pop(eng)
        deps.discard(o)
        o.deps = deps
        if dma is not None:
            self.dma_since.append(o)
            c = self.dma_keys.get(dma, 0) + 16 * ndma
            self.dma_keys[dma] = c
            o.dcount = c
        self.ops[eng].append(o)
        return o

    def emit(self, nc, stack, block):
        engs = {"pe": block.tensor, "act": block.scalar, "dve": block.vector,
                "pool": block.gpsimd, "sp": block.sync}
        csem = {e: stack.enter_context(nc.semaphore("c_" + e)) for e in engs}
        dsem = {k: stack.enter_context(nc.semaphore("d_%d" % i)) for i, k in enumerate(self.dma_keys)}
        for e, lst in self.ops.items():
            for o in lst:
                for d in o.deps:
                    if d.dma is not None:
                        continue
                    if d.eng != e or (e in self.SELF_SYNC):
                        d.sig = True
        for e, lst in self.ops.items():
            c = 0
            for o in lst:
                if o.dma is None and o.sig:
                    c += 1
                    o.sigidx = c
        nwait = [0]
        self.wlog = {}

        def make(e):
            lst = self.ops[e]

            def body(eng):
                seen = {}
                for o in lst:
                    need = {}
                    for d in o.deps:
                        if d.dma is not None:
                            key = ("d", d.dma); val = d.dcount
                        else:
                            if d.eng == e and e not in self.SELF_SYNC:
                                continue
                            key = ("c", d.eng); val = d.sigidx
                        if val > need.get(key, 0):
                            need[key] = val
                    for key, val in need.items():
                        if seen.get(key, 0) >= val:
                            continue
                        seen[key] = val
                        sem = dsem[key[1]] if key[0] == "d" else csem[key[1]]
                        eng.wait_ge(sem, val)
                        nwait[0] += 1
                        if not hasattr(o, "_w"):
                            self.wlog.setdefault(o.idx, []).append((key, val))
                    ins = o.fn(eng)
                    if o.dma is not None:
                        if not isinstance(ins, (list, tuple)):
                            ins = [ins]
                        assert len(ins) == o.ndma
                        for i_ in ins:
                            i_.then_inc(dsem[o.dma], 16)
                    elif o.sig:
                        ins.then_inc(csem[e], 1)
                if e == "sp":
                    for k, c in self.dma_keys.items():
                        if seen.get(("d", k), 0) < c:
                            eng.wait_ge(dsem[k], c)
            return body

        for e, reg in engs.items():
            reg(make(e))
        self.nwait = nwait


def t5_bucket_np(rel):
    n = np.maximum(rel, 0)
    max_exact = 16
    nf = np.maximum(n, max_exact).astype(np.float32)
    large = max_exact + (np.log(nf / np.float32(max_exact)) / np.float32(math.log(128 / max_exact))
                         * np.float32(32 - max_exact)).astype(np.int32)
    large = np.minimum(large, 31)
    return np.where(n < max_exact, n, large)


def host_consts(S):
    c = {}
    c["ident"] = np.eye(128, dtype=np.float32)
    inv = (10000.0 ** (-np.arange(0, 64, 2, dtype=np.float32) / 64)).astype(np.float32)
    pos = np.arange(S, dtype=np.float32)
    ang = pos[:, None] * inv[None, :]
    nch = S // 128
    c["cos"] = np.ascontiguousarray(np.cos(ang).astype(np.float32).reshape(nch, 128, 32).transpose(1, 0, 2))
    c["sin"] = np.ascontiguousarray(np.sin(ang).astype(np.float32).reshape(nch, 128, 32).transpose(1, 0, 2))
    gam = 1.0 - 2.0 ** (-5.0 - np.arange(8, dtype=np.float64))
    lg = np.log(gam)
    idx = np.arange(128, dtype=np.float64)
    rel = idx[None, :] - idx[:, None]
    dm = np.where(rel >= 0, np.exp(np.maximum(rel, 0)[None] * lg[:, None, None]), 0.0) * 0.125
    perm = [0, 2, 4, 6, 1, 3, 5, 7]
    c["dmatT"] = np.ascontiguousarray(dm[perm].transpose(1, 0, 2).astype(np.float32))
    zeta = np.exp((127 - idx)[:, None] * lg[None, :]) * 0.125
    c["zeta"] = np.ascontiguousarray(np.broadcast_to(zeta[:, :, None], (128, 8, 64))).astype(np.float32)
    xi = np.exp((idx + 1)[None, :] * lg[:, None])
    xif = np.zeros((128, 4, 128), np.float32)
    cd = np.zeros((128, 4, 64), np.float32)
    for p in range(4):
        for hl in range(2):
            xif[hl * 64:(hl + 1) * 64, p, :] = xi[2 * p + hl][None, :]
            cd[hl * 64:(hl + 1) * 64, p, :] = np.exp(128 * lg[2 * p + hl])
    c["xi"] = xif
    cdf = np.zeros((128, 4, 128), np.float32)
    bm4 = np.zeros((128, 4, 128), np.float32)
    for p in range(4):
        for hl in range(2):
            cdf[hl * 64:(hl + 1) * 64, p, :] = np.exp(128 * lg[2 * p + hl])
            bm4[hl * 64:(hl + 1) * 64, p, hl * 64:(hl + 1) * 64] = 1.0
    c["cd"] = cdf
    c["bm4"] = bm4
    i = np.arange(128)
    mk1 = np.zeros((128, 256), np.float32)
    mk1[:, :128] = (i[:, None] < i[None, :])
    mk1[:, 128:] = (i[:, None] <= i[None, :])
    c["mk1"] = mk1
    c["mk3"] = (i[None, :] < i[:, None]).astype(np.float32)
    bo = np.zeros((128, 128), np.float32)
    bo[:64, :64] = 1.0; bo[64:, 64:] = 1.0
    c["blockones"] = bo
    c["ones"] = np.ones((128, 128), np.float32)
    return c


def pack_cols(vecs):
    cols = []
    offs = []
    n = 0
    for v in vecs:
        v = np.asarray(v, np.float32).reshape(-1, 128)
        offs.append(n)
        n += v.shape[0]
        cols.append(v.T)
    return np.ascontiguousarray(np.concatenate(cols, axis=1)), offs


def build(S, layers, parts=("mix", "xattn", "mlp"), ncols=0, col_off=None):
    T = 512
    NT = S // T
    NCH = S // 128
    nc = bass.Bass("TRN2", target_bir_lowering=False)
    P = Prog()
    st = ExitStack()

    def din(name, shape, dt=F32):
        return nc.dram_tensor(name, list(shape), dt, kind="ExternalInput").ap()

    def dscr(name, shape, dt=F32):
        return nc.dram_tensor(name, list(shape), dt).ap()

    x_d = din("x", [S, D])
    mem_d = din("mem", [NMEM, D])
    out_d = nc.dram_tensor("out", [S, D], F32, kind="ExternalOutput").ap()
    cols_d = din("cols", [128, ncols])
    W = {}
    for nm, shp in (("xattn_w_q", [DEPTH, D, D]), ("xattn_w_kv", [DEPTH, D, 2 * D]), ("xattn_w_o", [DEPTH, D, D]),
                    ("mlp_w1", [DEPTH, D, 4 * D]), ("mlp_w2", [DEPTH, 4 * D, D]),
                    ("hyb_w_in", [2, D, HYB_COLS]), ("hyb_w_out", [2, D, D]),
                    ("diff_w_in", [2, D, 3 * D]), ("diff_w_out", [2, D, D]),
                    ("rwkv_w2", [2, 64, 512]), ("rwkv_a2", [2, 64, 512]), ("rwkv_g2", [2, 128, 512]),
                    ("lrows", [2, 128, 256]),
                    ("biasT", [128, 2, 8, 128]), ("cfar", [128, 8])):
        W[nm] = din(nm, shp)
    CT = {}
    hc = host_consts(S)
    for nm, arr in hc.items():
        CT[nm] = din("c_" + nm, arr.shape)

    hT = dscr("hT", [D, S])
    hnT = dscr("hnT", [D, S], BF16)
    qT_d = dscr("qT_d", [D, S], BF16)
    kT_d = dscr("kT_d", [D, S], BF16)
    v_d = dscr("v_d", [S, D], BF16)
    oT_d = dscr("oT_d", [D, S], BF16)
    hT_s = slots(NT); hnT_s = slots(NT); qT_s = slots(NT); kT_s = slots(NT); v_s = slots(NT)
    oT_s = [slots(NT) for _ in range(8)]

    def sb(name, shape, dt):
        return st.enter_context(nc.sbuf_tensor(name, list(shape), dt))

    NSLOT = 6
    arena = sb("arena", [128, NSLOT * 8192], BF16)
    arena_s = slots(NSLOT)
    colsT = sb("colsT", [128, max(ncols, 1)], F32); cols_s = Slot()
    ident = sb("ident", [128, 2, 128], F32); ident_s = Slot()
    identb = sb("identb", [128, 128], BF16); identb_s = Slot()
    onesb = sb("onesb", [128, 128], BF16); onesb_s = Slot()
    onesf = sb("onesf", [128, 128], F32); onesf_s = Slot()
    memn = sb("memn", [128, KC, NMEM], BF16); memn_s = Slot()
    WKN = 26880
    wk = sb("wk", [128, WKN], F32)
    ps = [st.enter_context(nc.psum_tensor("ps%d" % i, [128, 512], F32)) for i in range(8)]
    ps_s = [slots(4) for _ in range(8)]
    state = {"arena_next": 0, "off": 0}

    def reset():
        P.barrier()
        state["off"] = 0

    def alloc(shape, dt, nsl=1):
        n = 1
        for d_ in shape:
            n *= d_
        words = (n + 1) // 2 if dt == BF16 else n
        off = state["off"]
        assert off + words <= WKN, ("scratch overflow", off, words)
        state["off"] = off + words
        v = wk[:, off:off + words]
        if dt == BF16:
            v = v.bitcast(BF16)[:, 0:n]
        if len(shape) == 2:
            v = v.rearrange("p (a b) -> p a b", a=shape[0])
        elif len(shape) == 3:
            v = v.rearrange("p (a b c) -> p a b c", a=shape[0], b=shape[1])
        return v, (slots(nsl) if nsl > 1 else [Slot()])

    def psq(b, q0=0, q1=4):
        return ps_s[b][0:4]

    def dma(eng, out, in_, reads, writes, key):
        return P.add(eng, lambda e: e.dma_start(out=out, in_=in_), reads, writes, dma=key)

    def mm(out, lhsT, rhs, start, stop, reads, writes):
        return P.add("pe", lambda e: e.matmul(out, lhsT, rhs, start=start, stop=stop), reads, writes)

    def tr(out, in_, idt, reads, writes):
        return P.add("pe", lambda e: e.transpose(out, in_, idt), reads, writes)

    def act(out, in_, func, reads, writes, bias=None, scale=None):
        kw = {}
        if bias is not None:
            kw["bias"] = bias
        if scale is not None:
            kw["scale"] = scale
        return P.add("act", lambda e: e.activation(out=out, in_=in_, func=func, **kw), reads, writes)

    def amul(out, in_, c, reads, writes):
        return P.add("act", lambda e: e.mul(out=out, in_=in_, mul=c), reads, writes)

    def tt(eng, out, in0, in1, op, reads, writes):
        return P.add(eng, lambda e: e.tensor_tensor(out=out, in0=in0, in1=in1, op=op), reads, writes)

    def ts(eng, out, in0, s1, op0, reads, writes, s2=None, op1=None):
        if op1 is None:
            return P.add(eng, lambda e: e.tensor_scalar(out=out, in0=in0, scalar1=s1, scalar2=None, op0=op0),
                         reads, writes)
        return P.add(eng, lambda e: e.tensor_scalar(out=out, in0=in0, scalar1=s1, scalar2=s2, op0=op0, op1=op1),
                     reads, writes)

    def stt(eng, out, in0, scalar, in1, op0, op1, reads, writes):
        eng = "dve"
        return P.add(eng, lambda e: e.scalar_tensor_tensor(out=out, in0=in0, scalar=scalar, in1=in1,
                                                           op0=op0, op1=op1), reads, writes)

    def cp(eng, out, in_, reads, writes):
        if eng == "act":
            return P.add("act", lambda e: e.copy(out=out, in_=in_), reads, writes)
        return P.add(eng, lambda e: e.tensor_copy(out=out, in_=in_), reads, writes)

    def recip(out, in_, reads, writes):
        return P.add("dve", lambda e: e.reciprocal(out=out, in_=in_), reads, writes)

    def col(off, n=1):
        return colsT[:, off:off + n]

    def load_w(src_ap, kchunks, ncol_):
        need = -(-(kchunks * ncol_) // 8192)
        a0 = state["arena_next"]
        if a0 + need > NSLOT:
            a0 = 0
        state["arena_next"] = (a0 + need) % NSLOT
        view = arena[:, a0 * 8192: a0 * 8192 + kchunks * ncol_].rearrange("p (k n) -> p k n", k=kchunks)
        sl = arena_s[a0:a0 + need]
        srcv = src_ap.rearrange("(k p) n -> p k n", p=128)
        step = max(1, 8192 // ncol_)
        for k0 in range(0, kchunks, step):
            k1 = min(kchunks, k0 + step)
            s0 = (k0 * ncol_) // 8192
            s1 = (k1 * ncol_ - 1) // 8192
            dma("pool", view[:, k0:k1, :], srcv[:, k0:k1, :], [], sl[s0:s1 + 1], "w%d" % (a0 + s0))
        return view, sl

    dma("sp", colsT[:, 0:ncols], cols_d, [], [cols_s], "c0")
    dma("sp", ident[:, 0, :], CT["ident"], [], [ident_s], "c1")
    dma("sp", ident[:, 1, :], CT["ident"], [], [ident_s], "c1")
    dma("pool", identb[:], CT["ident"], [], [identb_s], "c2")
    dma("pool", onesb[:], CT["ones"], [], [onesb_s], "c3")
    dma("sp", onesf[:], CT["ones"], [], [onesf_s], "c4")
    id1 = ident[:, 0, :]

    def rmsnorm_tile(hb, hb_s, w_off, outb, outb_s, sq, sq_s, rs, rs_s, psb, n=T, out_dt_f32=False):
        P.add("pool", lambda e: e.tensor_tensor(out=sq[:, :, 0:n], in0=hb[:, :, 0:n], in1=hb[:, :, 0:n],
                                                op=ALU.mult), hb_s, sq_s)
        for k in range(KC):
            mm(ps[psb][:, 0:n], onesb[:], sq[:, k, 0:n], k == 0, k == KC - 1, sq_s + [onesb_s], psq(psb))
        act(rs[:, 0:n], ps[psb][:, 0:n], AF.Sqrt, psq(psb) + [cols_s], rs_s, bias=col(col_off["eps"]),
            scale=1.0 / D)
        recip(rs[:, 0:n], rs[:, 0:n], rs_s, rs_s)
        for k in range(KC):
            eng = "dve" if k % 2 == 0 else "pool"
            stt(eng, outb[:, k, 0:n], hb[:, k, 0:n], col(w_off + k), rs[:, 0:n],
                ALU.mult, ALU.mult, [hb_s[k]] + rs_s + [cols_s], [outb_s[k]])

    def load_h(tI, hb, hb_s, key):
        dma("sp", hb[:], hT[:, tI * T:(tI + 1) * T].rearrange("(c p) t -> p c t", p=128),
            [hT_s[tI]], hb_s, key)

    def store_h(tI, hb, hb_s, key):
        dma("sp", hT[:, tI * T:(tI + 1) * T].rearrange("(c p) t -> p c t", p=128), hb[:],
            hb_s, [hT_s[tI]], key)

    def ft(dram, tI):
        return dram[:, tI * T:(tI + 1) * T].rearrange("(c p) t -> p c t", p=128)

    def out_proj_add(wo, wo_s, src, src_s, nk, hb, hb_s, banks=(0, 1)):
        for j in range(KC):
            pb = banks[j % 2]
            for k in range(nk):
                mm(ps[pb][:, :], wo[:, k, j * 128:(j + 1) * 128], src[:, k, :], k == 0, k == nk - 1,
                   wo_s + [src_s[k]], psq(pb))
            tt("dve", hb[:, j, :], ps[pb][:, :], hb[:, j, :], ALU.add, psq(pb) + [hb_s[j]], [hb_s[j]])

    def phase_input():
        reset()
        hbuf = []; hbuf_s = []
        for i in range(2):
            a, s_ = alloc([KC, T], F32, KC); hbuf.append(a); hbuf_s.append(s_)
        xin = []; xin_s = []
        for i in range(2):
            a, s_ = alloc([D], F32); xin.append(a); xin_s.append(s_)
        for tI in range(NT):
            b = tI % 2
            for blk in range(4):
                g = tI * 4 + blk
                xb = g % 2
                dma("sp", xin[xb][:], x_d[g * 128:(g + 1) * 128, :], [], xin_s[xb], "xi%d" % xb)
                for c in range(KC):
                    tr(ps[c][:, blk * 128:(blk + 1) * 128], xin[xb][:, c * 128:(c + 1) * 128], id1,
                       xin_s[xb] + [ident_s], psq(c, blk, blk + 1))
            for c in range(KC):
                cp("act" if c % 2 == 0 else "dve", hbuf[b][:, c, :], ps[c][:, :], psq(c), [hbuf_s[b][c]])
            store_h(tI, hbuf[b], hbuf_s[b], "hs%d" % b)

    def phase_mem():
        reset()
        mtm, mtm_s = alloc([2 * D], F32)
        mf, mf_s = alloc([KC, NMEM], F32, KC)
        sq, sq_s = alloc([KC, NMEM], BF16)
        rs, rs_s = alloc([NMEM], F32)
        tmpo, tmpo_s = alloc([KC, NMEM], BF16, KC)
        for mb in range(2):
            dma("sp", mtm[:, mb * D:(mb + 1) * D], mem_d[mb * 128:(mb + 1) * 128, :], [], mtm_s, "hl0")
        for c in range(KC):
            for mb in range(2):
                tr(ps[c % 4][:, mb * 128:(mb + 1) * 128], mtm[:, mb * D + c * 128: mb * D + (c + 1) * 128], id1,
                   mtm_s + [ident_s], psq(c % 4, mb, mb + 1))
            cp("act" if c % 2 == 0 else "dve", mf[:, c, :], ps[c % 4][:, 0:NMEM], psq(c % 4, 0, 2), [mf_s[c]])
        rmsnorm_tile(mf, mf_s, col_off["mem_norm_w"], tmpo, tmpo_s, sq, sq_s, rs, rs_s, 4, n=NMEM)
        for k in range(KC):
            cp("pool", memn[:, k, :], tmpo[:, k, :], [tmpo_s[k]], [memn_s])

    def phase_xattn(layer):
        reset()
        wq, wq_s = load_w(W["xattn_w_q"][layer], KC, D)
        wkv, wkv_s = load_w(W["xattn_w_kv"][layer], KC, 2 * D)
        wo, wo_s = load_w(W["xattn_w_o"][layer], KC, D)
        kTx, kTx_s = alloc([KC, NMEM], BF16)
        vx, vx_s = alloc([2, D], BF16)
        sq, sq_s = alloc([KC, T], BF16)
        pexp, _ = alloc([2, 2, T], BF16)
        pexp_s = [slots(2) for _ in range(2)]
        hbuf = []; hbuf_s = []; hnb = []; hnb_s = []; rstd = []; rstd_s = []; rden = []; rden_s = []
        for i in range(2):
            a, s_ = alloc([KC, T], F32, KC); hbuf.append(a); hbuf_s.append(s_)
            a, s_ = alloc([KC, T], BF16, KC); hnb.append(a); hnb_s.append(s_)
            a, s_ = alloc([T], F32); rstd.append(a); rstd_s.append(s_)
            a, s_ = alloc([T], F32); rden.append(a); rden_s.append(s_)
        qo, qo_s = alloc([16, T], BF16, 16)
        for j in range(KC):
            b = j % 2
            for k in range(KC):
                mm(ps[b][:, 0:NMEM], wkv[:, k, j * 128:(j + 1) * 128], memn[:, k, :], k == 0, k == KC - 1,
                   wkv_s + [memn_s], psq(b, 0, 2))
            cp("act" if j % 2 == 0 else "dve", kTx[:, j, :], ps[b][:, 0:NMEM], psq(b, 0, 2), kTx_s)
        for mb in range(2):
            for hf in range(2):
                b = 2 + hf
                for k in range(KC):
                    mm(ps[b][:, :], memn[:, k, mb * 128:(mb + 1) * 128], wkv[:, k, D + hf * 512: D + (hf + 1) * 512],
                       k == 0, k == KC - 1, wkv_s + [memn_s], psq(b))
                cp("act" if hf == 0 else "dve", vx[:, mb, hf * 512:(hf + 1) * 512], ps[b][:, :], psq(b), vx_s)
        for tI in range(NT):
            b = tI % 2
            hb = hbuf[b]; hb_s = hbuf_s[b]
            load_h(tI, hb, hb_s, "hl%d" % b)
            rmsnorm_tile(hb, hb_s, col_off["norm_cross_w"] + layer * KC, hnb[b], hnb_s[b], sq, sq_s, rstd[b],
                         rstd_s[b], 7)
            for j in range(KC):
                pb = j % 2
                for k in range(KC):
                    mm(ps[pb][:, :], wq[:, k, j * 128:(j + 1) * 128], hnb[b][:, k, :], k == 0, k == KC - 1,
                       wq_s + [hnb_s[b][k]], psq(pb))
                if j % 2 == 0:
                    amul(qo[:, j, :], ps[pb][:, :], 1.0 / 16.0, psq(pb), [qo_s[j]])
                else:
                    ts("dve", qo[:, j, :], ps[pb][:, :], 1.0 / 16.0, ALU.mult, psq(pb), [qo_s[j]])
            for hd in range(4):
                hp = hd % 2
                for mb in range(2):
                    pb = 2 + mb
                    for jj in range(2):
                        j = 2 * hd + jj
                        mm(ps[pb][:, :], kTx[:, j, mb * 128:(mb + 1) * 128], qo[:, j, :], jj == 0, jj == 1,
                           kTx_s + [qo_s[j]], psq(pb))
                    act(pexp[:, hp, mb, :], ps[pb][:, :], AF.Exp, psq(pb), [pexp_s[hp][mb]])
                for mb in range(2):
                    mm(ps[4][:, :], onesb[:], pexp[:, hp, mb, :], mb == 0, mb == 1,
                       [onesb_s, pexp_s[hp][mb]], psq(4))
                recip(rden[hp][:], ps[4][:, :], psq(4), rden_s[hp])
                for jj in range(2):
                    j = 2 * hd + jj
                    pb = 5 + jj
                    for mb in range(2):
                        mm(ps[pb][:, :], vx[:, mb, j * 128:(j + 1) * 128], pexp[:, hp, mb, :], mb == 0, mb == 1,
                           vx_s + [pexp_s[hp][mb]], psq(pb))
                    tt("dve", qo[:, 8 + j, :], ps[pb][:, :], rden[hp][:], ALU.mult, psq(pb) + rden_s[hp],
                       [qo_s[8 + j]])
            out_proj_add(wo, wo_s, qo[:, 8:16, :], qo_s[8:16], KC, hb, hb_s)
            store_h(tI, hb, hb_s, "hs%d" % b)

    def phase_mlp(layer, half):
        reset()
        w1, w1_s = load_w(W["mlp_w1"][layer][:, half * 2048:(half + 1) * 2048], KC, 2048)
        w2, w2_s = load_w(W["mlp_w2"][layer][half * 2048:(half + 1) * 2048, :], 16, D)
        sq, sq_s = alloc([KC, T], BF16)
        hbuf = []; hbuf_s = []; hnb = []; hnb_s = []; rstd = []; rstd_s = []; relu_t = []; relu_s = []
        h1 = []; h1_s = []
        for i in range(2):
            a, s_ = alloc([KC, T], F32, KC); hbuf.append(a); hbuf_s.append(s_)
            a, s_ = alloc([KC, T], BF16, KC); hnb.append(a); hnb_s.append(s_)
            a, s_ = alloc([T], F32); rstd.append(a); rstd_s.append(s_)
            a, s_ = alloc([T], F32); relu_t.append(a); relu_s.append(s_)
            a, s_ = alloc([16, T], BF16, 16); h1.append(a); h1_s.append(s_)
        for tI in range(NT):
            b = tI % 2
            hb = hbuf[b]; hb_s = hbuf_s[b]
            load_h(tI, hb, hb_s, "hl%d" % b)
            if half == 0:
                rmsnorm_tile(hb, hb_s, col_off["norm_mlp_w"] + layer * KC, hnb[b], hnb_s[b], sq, sq_s, rstd[b],
                             rstd_s[b], 7)
                dma("sp", ft(hnT, tI), hnb[b][:], hnb_s[b], [hnT_s[tI]], "hns%d" % b)
            else:
                dma("sp", hnb[b][:], ft(hnT, tI), [hnT_s[tI]], hnb_s[b], "hnl%d" % b)
            for f in range(16):
                pb = f % 2
                for k in range(KC):
                    mm(ps[pb][:, :], w1[:, k, f * 128:(f + 1) * 128], hnb[b][:, k, :], k == 0, k == KC - 1,
                       w1_s + [hnb_s[b][k]], psq(pb))
                act(relu_t[pb][:], ps[pb][:, :], AF.Relu, psq(pb), relu_s[pb])
                tt("pool" if f % 2 == 0 else "dve", h1[b][:, f, :], relu_t[pb][:], relu_t[pb][:], ALU.mult,
                   relu_s[pb], [h1_s[b][f]])
            out_proj_add(w2, w2_s, h1[b], h1_s[b], 16, hb, hb_s, banks=(2, 3))
            store_h(tI, hb, hb_s, "hs%d" % b)

    def phase_final():
        reset()
        sq, sq_s = alloc([KC, T], BF16)
        hbuf = []; hbuf_s = []; rstd = []; rstd_s = []; otm = []; otm_s = []
        for i in range(2):
            a, s_ = alloc([KC, T], F32, KC); hbuf.append(a); hbuf_s.append(s_)
            a, s_ = alloc([T], F32); rstd.append(a); rstd_s.append(s_)
            a, s_ = alloc([D], F32); otm.append(a); otm_s.append(s_)
        fno, fno_s = alloc([KC, T], F32, KC)
        for tI in range(NT):
            b = tI % 2
            hb = hbuf[b]; hb_s = hbuf_s[b]
            load_h(tI, hb, hb_s, "hl%d" % b)
            P.add("pool", lambda e, hb=hb: e.tensor_tensor(out=sq[:, :, :], in0=hb[:, :, :], in1=hb[:, :, :],
                                                           op=ALU.mult), hb_s, sq_s)
            for k in range(KC):
                mm(ps[7][:, :], onesb[:], sq[:, k, :], k == 0, k == KC - 1, sq_s + [onesb_s], psq(7))
            act(rstd[b][:], ps[7][:, :], AF.Sqrt, psq(7) + [cols_s], rstd_s[b], bias=col(col_off["eps"]),
                scale=1.0 / D)
            recip(rstd[b][:], rstd[b][:], rstd_s[b], rstd_s[b])
            for k in range(KC):
                stt("dve" if k % 2 == 0 else "pool", fno[:, k, :], hb[:, k, :], col(col_off["final_norm_w"] + k),
                    rstd[b][:], ALU.mult, ALU.mult, [hb_s[k], cols_s] + rstd_s[b], [fno_s[k]])
            for blk in range(4):
                ob = blk % 2
                for half in range(2):
                    for cc in range(4):
                        c = half * 4 + cc
                        tr(ps[half][:, cc * 128:(cc + 1) * 128], fno[:, c, blk * 128:(blk + 1) * 128], id1,
                           [fno_s[c], ident_s], psq(half, cc, cc + 1))
                    cp("act" if half == 0 else "dve", otm[ob][:, half * 512:(half + 1) * 512], ps[half][:, :],
                       psq(half), otm_s[ob])
                g = tI * 4 + blk
                dma("sp", out_d[g * 128:(g + 1) * 128, :], otm[ob][:], otm_s[ob], [], "os%d" % ob)

    def phase_diff(layer):
        i = layer // 2
        lam_init = 0.8 - 0.6 * math.exp(-0.3 * layer)
        reset()
        win, win_s = load_w(W["diff_w_in"][i], KC, 3 * D)
        sq, sq_s = alloc([KC, T], BF16)
        hb, hb_s = alloc([KC, T], F32, KC)
        hn, hn_s = alloc([KC, T], BF16, KC)
        rs, rs_s = alloc([T], F32)
        qb = []; qb_s = []; kb_ = []; kb_s = []; vb = []; vb_s = []
        for j in range(2):
            a, s_ = alloc([KC, T], BF16, KC); qb.append(a); qb_s.append(s_)
            a, s_ = alloc([KC, T], BF16, KC); kb_.append(a); kb_s.append(s_)
            a, s_ = alloc([4, D], BF16, 4); vb.append(a); vb_s.append(s_)
        for tI in range(NT):
            b = tI % 2
            load_h(tI, hb, hb_s, "hl0")
            rmsnorm_tile(hb, hb_s, col_off["norm_mix_w"] + layer * KC, hn, hn_s, sq, sq_s, rs, rs_s, 7)
            for j in range(KC):
                for which in range(2):
                    pb = which
                    for k in range(KC):
                        mm(ps[pb][:, :], win[:, k, which * D + j * 128: which * D + (j + 1) * 128], hn[:, k, :],
                           k == 0, k == KC - 1, win_s + [hn_s[k]], psq(pb))
                    if which == 0:
                        amul(qb[b][:, j, :], ps[pb][:, :], 0.125, psq(pb), [qb_s[b][j]])
                    else:
                        cp("dve", kb_[b][:, j, :], ps[pb][:, :], psq(pb), [kb_s[b][j]])
            for blk in range(4):
                for hf in range(2):
                    pb = 2 + hf
                    for k in range(KC):
                        mm(ps[pb][:, :], hn[:, k, blk * 128:(blk + 1) * 128],
                           win[:, k, 2 * D + hf * 512: 2 * D + (hf + 1) * 512], k == 0, k == KC - 1,
                           win_s + [hn_s[k]], psq(pb))
                    cp("act" if hf == 0 else "dve", vb[b][:, blk, hf * 512:(hf + 1) * 512], ps[pb][:, :], psq(pb),
                       [vb_s[b][blk]])
            dma("sp", ft(qT_d, tI), qb[b][:], qb_s[b], [qT_s[tI]], "dq%d" % b)
            dma("sp", ft(kT_d, tI), kb_[b][:], kb_s[b], [kT_s[tI]], "dk%d" % b)
            dma("sp", v_d[tI * T:(tI + 1) * T, :].rearrange("(c p) n -> p c n", p=128), vb[b][:], vb_s[b],
                [v_s[tI]], "dv%d" % b)
        reset()
        lrow, lrow_s = alloc([256], F32)
        dma("sp", lrow[:], W["lrows"][i], [], lrow_s, "kd0")
        bias_t, bias_s = alloc([2, 8, 128], F32)
        dma("sp", bias_t[:], W["biasT"], [], bias_s, "kd1")
        cf, cf_s = alloc([8], F32)
        dma("sp", cf[:], W["cfar"], [], cf_s, "kd2")
        lt, lt_s = alloc([128], F32)
        lam, lam_s = alloc([4], F32)
        tt("dve", lt[:, 0:64], lrow[:, 0:64], lrow[:, 64:128], ALU.mult, lrow_s, lt_s)
        tt("dve", lt[:, 64:128], lrow[:, 128:192], lrow[:, 192:256], ALU.mult, lrow_s, lt_s)
        P.add("dve", lambda e: e.tensor_reduce(out=lam[:, 0:2], in_=lt[:].rearrange("p (a b) -> p a b", a=2),
                                               axis=AX.X, op=ALU.add), lt_s, lam_s)
        act(lam[:, 0:2], lam[:, 0:2], AF.Exp, lam_s, lam_s)
        tt("dve", lam[:, 2:3], lam[:, 1:2], lam[:, 0:1], ALU.subtract, lam_s, lam_s)
        ts("dve", lam[:, 2:3], lam[:, 2:3], -lam_init, ALU.add, lam_s, lam_s)
        kTh = []; kTh_s = []; vh = []; vh_s = []
        for j in range(2):
            a, s_ = alloc([S], BF16); kTh.append(a); kTh_s.append(s_)
            a, s_ = alloc([NCH, 128], BF16); vh.append(a); vh_s.append(s_)
        qt_ = []; qt_s = []; pt = []; pt_s = []
        for j in range(2):
            a, s_ = alloc([T], BF16); qt_.append(a); qt_s.append(s_)
        for j in range(4):
            a, s_ = alloc([T], BF16, 4); pt.append(a); pt_s.append(s_)
        ntmp = []; ntmp_s = []
        for j in range(2):
            a, s_ = alloc([128], F32); ntmp.append(a); ntmp_s.append(s_)
        rd, rd_s = alloc([T], F32)
        o1, o1_s = alloc([T], F32)
        o2, o2_s = alloc([T], F32)
        osq, osq_s = alloc([T], F32)
        ot = []; ot_s = []
        for j in range(2):
            a, s_ = alloc([T], BF16); ot.append(a); ot_s.append(s_)
        it = 0
        for h in range(8):
            hbuf_i = h % 2
            dma("sp", kTh[hbuf_i][:], kT_d[h * 128:(h + 1) * 128, :], kT_s, kTh_s[hbuf_i], "dk%d" % hbuf_i)
            vsrc = v_d[:, h * 128:(h + 1) * 128].rearrange("(c p) n -> p c n", p=128)
            vdst = vh[hbuf_i]
            ngrp = max(1, NCH // 2)
            P.add("sp", lambda e, vsrc=vsrc, vdst=vdst, ngrp=ngrp: [
                e.dma_start(out=vdst[:, 2 * g_:2 * g_ + 2, :], in_=vsrc[:, 2 * g_:2 * g_ + 2, :]) for g_ in range(ngrp)],
                v_s, vh_s[hbuf_i], dma="dv%d" % hbuf_i, ndma=ngrp)
            KT = kTh[hbuf_i]; VH = vh[hbuf_i]
            for qt in range(NT):
                qb_i = it % 2; it += 1
                QT = qt_[qb_i]
                dma("sp", QT[:], qT_d[h * 128:(h + 1) * 128, qt * T:(qt + 1) * T], [qT_s[qt]], qt_s[qb_i],
                    "dq%d" % qb_i)
                nkb = 4 * qt + 4
                for kb in range(nkb):
                    j = kb - 4 * qt
                    q0 = max(j, 0)
                    c0 = q0 * 128
                    par = kb % 2
                    for comp in range(2):
                        rows = slice(comp * 64, comp * 64 + 64)
                        pb = par * 2 + comp
                        mm(ps[pb][:, c0:T], KT[rows, kb * 128:(kb + 1) * 128], QT[rows, c0:T], True, True,
                           kTh_s[hbuf_i] + qt_s[qb_i], psq(pb, q0, 4))
                        PT = pt[par * 2 + comp]; PT_s = pt_s[par * 2 + comp]
                        far0 = 4
                        for qs in range(q0, 4):
                            dist = 4 * qt + qs - kb
                            if dist >= 2:
                                far0 = qs
                                break
                            nt_ = ntmp[comp]
                            tt("dve", nt_[:], ps[pb][:, qs * 128:(qs + 1) * 128], bias_t[:, dist, h, :], ALU.add,
                               psq(pb, qs, qs + 1) + bias_s, ntmp_s[comp])
                            act(PT[:, qs * 128:(qs + 1) * 128], nt_[:], AF.Exp, ntmp_s[comp], [PT_s[qs]])
                        if far0 < 4:
                            act(PT[:, far0 * 128:T], ps[pb][:, far0 * 128:T], AF.Exp, psq(pb, far0, 4) + cf_s,
                                PT_s[far0:4], bias=cf[:, h:h + 1])
                    for comp in range(2):
                        PT = pt[par * 2 + comp]; PT_s = pt_s[par * 2 + comp]
                        mm(ps[4 + comp][:, c0:T], VH[:, kb, :], PT[:, c0:T], kb == 0, kb == nkb - 1,
                           vh_s[hbuf_i] + PT_s[q0:4], psq(4 + comp))
                        mm(ps[6 + comp][:, c0:T], onesb[:], PT[:, c0:T], kb == 0, kb == nkb - 1,
                           [onesb_s] + PT_s[q0:4], psq(6 + comp))
                recip(rd[:], ps[6][:, :], psq(6), rd_s)
                tt("dve", o1[:], ps[4][:, :], rd[:], ALU.mult, psq(4) + rd_s, o1_s)
                recip(rd[:], ps[7][:, :], psq(7), rd_s)
                tt("dve", o2[:], ps[5][:, :], rd[:], ALU.mult, psq(5) + rd_s, o2_s)
                stt("dve", o1[:], o2[:], lam[:, 2:3], o1[:], ALU.mult, ALU.add, o2_s + o1_s + lam_s, o1_s)
                tt("pool", osq[:], o1[:], o1[:], ALU.mult, o1_s, osq_s)
                mm(ps[0][:, :], onesf[:], osq[:], True, True, [onesf_s] + osq_s, psq(0))
                act(rd[:], ps[0][:, :], AF.Sqrt, psq(0) + [cols_s], rd_s, bias=col(col_off["eps"]), scale=1.0 / 128)
                recip(rd[:], rd[:], rd_s, rd_s)
                ts("dve", rd[:], rd[:], 1.0 - lam_init, ALU.mult, rd_s, rd_s)
                OT = ot[qb_i]
                stt("dve", OT[:], o1[:], col(col_off["diff_ln_w"] + i), rd[:], ALU.mult, ALU.mult,
                    o1_s + rd_s + [cols_s], ot_s[qb_i])
                dma("sp", oT_d[h * 128:(h + 1) * 128, qt * T:(qt + 1) * T], OT[:], ot_s[qb_i], [oT_s[h][qt]],
                    "do%d" % qb_i)
        reset()
        wo, wo_s = load_w(W["diff_w_out"][i], KC, D)
        hbuf = []; hbuf_s = []; ob = []; ob_s = []
        for j in range(2):
            a, s_ = alloc([KC, T], F32, KC); hbuf.append(a); hbuf_s.append(s_)
            a, s_ = alloc([KC, T], BF16, KC); ob.append(a); ob_s.append(s_)
        for tI in range(NT):
            b = tI % 2
            load_h(tI, hbuf[b], hbuf_s[b], "hl%d" % b)
            dma("sp", ob[b][:], ft(oT_d, tI), [oT_s[h][tI] for h in range(8)], ob_s[b], "hnl%d" % b)
            out_proj_add(wo, wo_s, ob[b], ob_s[b], KC, hbuf[b], hbuf_s[b])
            store_h(tI, hbuf[b], hbuf_s[b], "hs%d" % b)

    def phase_hyb(layer):
        i = layer // 2
        co = col_off
        hyb_a(layer)
        hyb_b(layer)

    def hyb_a(layer):
        i = layer // 2
        co = col_off
        reset()
        wA, wA_s = load_w(W["hyb_w_in"][i][:, 0:A_COLS], KC, A_COLS)
        lw2, lw2_s = alloc([512], BF16)
        g2, g2_s = alloc([512], BF16)
        dma("pool", lw2[0:64, :], W["rwkv_w2"][i], [], lw2_s, "c2")
        dma("pool", lw2[64:128, :], W["rwkv_a2"][i], [], lw2_s, "c2")
        dma("pool", g2[:], W["rwkv_g2"][i], [], g2_s, "c3")
        mk1, mk1_s = alloc([2, 256], F32)
        mk3, mk3_s = alloc([2, 128], F32)
        bo, bo_s = alloc([128], F32)
        for hl in range(2):
            dma("sp", mk1[:, hl, :], CT["mk1"], [], mk1_s, "ka0")
            dma("sp", mk3[:, hl, :], CT["mk3"], [], mk3_s, "ka1")
        dma("sp", bo[:], CT["blockones"], [], bo_s, "ka2")
        hb, hb_s = alloc([KC, T], F32, KC)
        hn, hn_s = alloc([KC, T], BF16, KC)
        sq, sq_s = alloc([KC, T], BF16)
        rs, rs_s = alloc([T], F32)
        carry, carry_s = alloc([16], F32)
        NC = hb[:, 7, 0:256].rearrange("p (a b) -> p a b", a=2)
        NC_s = [hb_s[7]]
        NREF = int(os.environ.get("NREF", "2"))
        P.add("pool", lambda e: e.memset(carry[:], 0.0), [], carry_s)
        STt, ST_s = alloc([4, 64], F32, 4)
        P.add("pool", lambda e: e.memset(STt[:], 0.0), [], ST_s)
        PA, PA_s = alloc([3, T + 1], F32, 3)
        LA, LA_s = alloc([T], BF16)
        SGD, SGD_s = alloc([T], BF16)
        names = ["LW", "L", "eL", "eLm", "enL", "AI", "G", "kx", "t1", "kk", "km", "BH", "KH", "BTf", "KTf", "BON",
                 "YF"]
        Wt = {}; Ws = {}
        for nm in names:
            Wt[nm], Ws[nm] = alloc([T], F32)
        AR, AR_s = alloc([4, 256], F32)
        TM, TM_s = alloc([3, 128], F32)
        MP = []; MP_s = []; MT = []; MT_s = []
        for j in range(2):
            a, s_ = alloc([2, 256], F32, 2); MP.append(a); MP_s.append(s_)
            a, s_ = alloc([2, 128], F32); MT.append(a); MT_s.append(s_)
        ARB, ARB_s = alloc([2, 128], F32)
        E2, E2_s = alloc([2, 256], F32)
        TT_, TT_s = alloc([2, 128], F32)
        dtmp, dtmp_s = Wt["t1"], Ws["t1"]
        XS, XS_s = alloc([128], F32)
        UU, UU_s = alloc([128], F32)
        YT, YT_s = alloc([128], F32)
        YQ, YQ_s = alloc([128], F32)
        st4, st4_s = alloc([8], F32)
        YA, YA_s = alloc([4, T], BF16, 4)
        rows2 = [slice(0, 64), slice(64, 128)]

        def proj_chunk(c, dst, dst_s, pb):
            for k in range(KC):
                mm(ps[pb][:, :], wA[:, k, c * 128:(c + 1) * 128], hn[:, k, :], k == 0, k == KC - 1,
                   wA_s + [hn_s[k]], psq(pb))
            cp("act", dst[:, 1:T + 1], ps[pb][:, :], psq(pb), dst_s)
            cp("pool", dst[:, 0:1], carry[:, c:c + 1], carry_s + dst_s, dst_s)
            tt("dve", dtmp[:], dst[:, 0:T], dst[:, 1:T + 1], ALU.subtract, dst_s, dtmp_s)
            cp("pool", carry[:, c:c + 1], dst[:, T:T + 1], dst_s + carry_s, carry_s)
            stt("dve", dst[:, 1:T + 1], dtmp[:], col(co["rwkv_mu"] + i * 14 + c), dst[:, 1:T + 1], ALU.mult, ALU.add,
                dtmp_s + dst_s + [cols_s], dst_s)

        for tI in range(NT):
            load_h(tI, hb, hb_s, "hl0")
            rmsnorm_tile(hb, hb_s, co["norm_mix_w"] + layer * KC, hn, hn_s, sq, sq_s, rs, rs_s, 7)
            dma("sp", ft(hnT, tI), hn[:], hn_s, [hnT_s[tI]], "hns0")
            if "noa" in parts:
                continue
            proj_chunk(12, PA[:, 0, :], [PA_s[0]], 0)
            act(LA[0:64, :], PA[0:64, 0, 1:T + 1], AF.Tanh, [PA_s[0]], LA_s)
            cp("dve", LA[64:128, :], PA[64:128, 0, 1:T + 1], [PA_s[0]], LA_s)
            proj_chunk(13, PA[:, 1, :], [PA_s[1]], 1)
            act(SGD[:], PA[:, 1, 1:T + 1], AF.Sigmoid, [PA_s[1]], SGD_s)
            for g in range(4):
                proj_chunk(g, PA[:, 0, :], [PA_s[0]], 0)
                proj_chunk(4 + g, PA[:, 1, :], [PA_s[1]], 1)
                proj_chunk(8 + g, PA[:, 2, :], [PA_s[2]], 0)
                R_ = PA[:, 0, 1:T + 1]; K_ = PA[:, 1, 1:T + 1]; V_ = PA[:, 2, 1:T + 1]
                cg_ = i * 4 + g
                mm(ps[1][:, :], lw2[0:64, g * 128:(g + 1) * 128], LA[0:64, :], True, True, lw2_s + LA_s, psq(1))
                act(Wt["LW"][:], ps[1][:, :], AF.Sigmoid, psq(1) + [cols_s], Ws["LW"], bias=col(co["rwkv_w0"] + cg_))
                ts("pool", Wt["LW"][:], Wt["LW"][:], -0.6065306597126334, ALU.mult, Ws["LW"], Ws["LW"])
                mm(ps[0][:, :], lw2[64:128, g * 128:(g + 1) * 128], LA[64:128, :], True, True, lw2_s + LA_s, psq(0))
                act(Wt["AI"][:], ps[0][:, :], AF.Sigmoid, psq(0) + [cols_s], Ws["AI"], bias=col(co["rwkv_a0"] + cg_))
                mm(ps[1][:, :], g2[:, g * 128:(g + 1) * 128], SGD[:], True, True, g2_s + SGD_s, psq(1))
                cp("act", Wt["G"][:], ps[1][:, :], psq(1), Ws["G"])
                for ch in range(4):
                    cs = slice(ch * 128, (ch + 1) * 128)
                    P.add("dve", lambda e, cs=cs: e.tensor_tensor_scan(
                        out=Wt["L"][:, cs], data0=onesf[:], data1=Wt["LW"][:, cs], initial=0.0,
                        op0=ALU.mult, op1=ALU.add), Ws["LW"] + [onesf_s], Ws["L"])
                act(Wt["eL"][:], Wt["L"][:], AF.Exp, Ws["L"], Ws["eL"])
                act(Wt["enL"][:], Wt["L"][:], AF.Exp, Ws["L"], Ws["enL"], scale=-1.0)
                tt("pool", Wt["t1"][:], Wt["L"][:], Wt["LW"][:], ALU.subtract, Ws["L"] + Ws["LW"], Ws["t1"])
                act(Wt["eLm"][:], Wt["t1"][:], AF.Exp, Ws["t1"], Ws["eLm"])
                ts("pool", Wt["kx"][:], K_, col(co["rwkv_k_k"] + cg_), ALU.mult, [PA_s[1], cols_s], Ws["kx"])
                tt("pool", Wt["t1"][:], Wt["kx"][:], Wt["kx"][:], ALU.mult, Ws["kx"], Ws["t1"])
                mm(ps[0][:, :], bo[:], Wt["t1"][:], True, True, bo_s + Ws["t1"], psq(0))
                act(Wt["kk"][:], ps[0][:, :], AF.Sqrt, psq(0), Ws["kk"])
                recip(Wt["kk"][:], Wt["kk"][:], Ws["kk"], Ws["kk"])
                tt("dve", Wt["kk"][:], Wt["kk"][:], Wt["kx"][:], ALU.mult, Ws["kk"] + Ws["kx"], Ws["kk"])
                ts("pool", Wt["t1"][:], Wt["AI"][:], -1.0, ALU.add, Ws["AI"] + [cols_s], Ws["t1"],
                   s2=col(co["rwkv_k_a"] + cg_), op1=ALU.mult)
                stt("dve", Wt["km"][:], Wt["t1"][:], 1.0, K_, ALU.add, ALU.mult, Ws["t1"] + [PA_s[1]], Ws["km"])
                kk3 = Wt["kk"][:].rearrange("p (c t) -> p c t", c=4)
                stt("dve", AR[:, :, 0:128], kk3, -1.0, Wt["eLm"][:].rearrange("p (c t) -> p c t", c=4),
                    ALU.mult, ALU.mult, Ws["kk"] + Ws["eLm"], AR_s)
                tt("pool", AR[:, :, 128:256], R_.rearrange("p (c t) -> p c t", c=4),
                   Wt["eL"][:].rearrange("p (c t) -> p c t", c=4), ALU.mult, [PA_s[0]] + Ws["eL"], AR_s)
                tt("pool", Wt["BH"][:], Wt["kk"][:], Wt["AI"][:], ALU.mult, Ws["kk"] + Ws["AI"], Ws["BH"])
                tt("pool", Wt["BH"][:], Wt["BH"][:], Wt["enL"][:], ALU.mult, Ws["BH"] + Ws["enL"], Ws["BH"])
                tt("dve", Wt["KH"][:], Wt["km"][:], Wt["enL"][:], ALU.mult, Ws["km"] + Ws["enL"], Ws["KH"])
                for ch in range(4):
                    cs = slice(ch * 128, (ch + 1) * 128)
                    gc = Wt["eL"][:, ch * 128 + 127: ch * 128 + 128]
                    ts("pool", Wt["BTf"][:, cs], Wt["BH"][:, cs], gc, ALU.mult, Ws["BH"] + Ws["eL"], Ws["BTf"])
                    ts("dve", Wt["KTf"][:, cs], Wt["KH"][:, cs], gc, ALU.mult, Ws["KH"] + Ws["eL"], Ws["KTf"])
                stt("pool", Wt["t1"][:], R_, col(co["rwkv_r_k"] + cg_), Wt["km"][:], ALU.mult, ALU.mult,
                    [PA_s[0], cols_s] + Ws["km"], Ws["t1"])
                mm(ps[1][:, :], bo[:], Wt["t1"][:], True, True, bo_s + Ws["t1"], psq(1))
                tt("dve", Wt["BON"][:], ps[1][:, :], V_, ALU.mult, psq(1) + [PA_s[2]], Ws["BON"])
                for ch in range(4):
                    cs = slice(ch * 128, (ch + 1) * 128)
                    tr(ps[2][:, 0:128], V_[:, cs], id1, [PA_s[2], ident_s], psq(2, 0, 1))
                    tr(ps[2][:, 128:256], Wt["BTf"][:, cs], id1, Ws["BTf"] + [ident_s], psq(2, 1, 2))
                    tr(ps[2][:, 256:384], Wt["KTf"][:, cs], id1, Ws["KTf"] + [ident_s], psq(2, 2, 3))
                    cp("act", TM[:].rearrange("p a b -> p (a b)"), ps[2][:, 0:384], psq(2, 0, 3), TM_s)
                    for hl in range(2):
                        r_ = rows2[hl]
                        mm(ps[3][:, hl * 256:(hl + 1) * 256], Wt["BH"][r_, cs], AR[r_, ch, :], True, True,
                           Ws["BH"] + AR_s, psq(3, 2 * hl, 2 * hl + 2))
                        mm(ps[4][:, hl * 256:(hl + 1) * 256], Wt["KH"][r_, cs], AR[r_, ch, :], True, True,
                           Ws["KH"] + AR_s, psq(4, 2 * hl, 2 * hl + 2))
                        mm(ps[5][:, hl * 128:(hl + 1) * 128], AR[r_, ch, 0:128], Wt["BH"][r_, cs], True, True,
                           Ws["BH"] + AR_s, psq(5, hl, hl + 1))
                    v3 = ps[3][:, :].rearrange("p (a b) -> p a b", a=2)
                    cur = 0
                    tt("dve", MP[cur][:, :, 0:128], v3[:, :, 0:128], mk1[:, :, 0:128], ALU.mult, psq(3) + mk1_s,
                       MP_s[cur])
                    cp("pool", NC, MP[cur][:, :, 0:128], MP_s[cur], NC_s)
                    tt("dve", ARB[:], v3[:, :, 128:256], mk1[:, :, 128:256], ALU.mult, psq(3) + mk1_s, ARB_s)
                    cp("pool", MP[cur][:, :, 128:256], ident[:], [ident_s], MP_s[cur])
                    tt("dve", E2[:], ps[4][:, :].rearrange("p (a b) -> p a b", a=2), mk1[:], ALU.mult,
                       psq(4) + mk1_s, E2_s)
                    tt("dve", MT[cur][:], ps[5][:, 0:256].rearrange("p (a b) -> p a b", a=2), mk3[:], ALU.mult,
                       psq(5, 0, 2) + mk3_s, MT_s[cur])
                    for j in range(6):
                        nxt = 1 - cur
                        pX = 3 + (j % 2)
                        for hl in range(2):
                            if j < 5:
                                mm(ps[pX][:, hl * 256:(hl + 1) * 256], MT[cur][:, hl, :], MP[cur][:, hl, :], True, True,
                                   MT_s[cur] + MP_s[cur], psq(pX, 2 * hl, 2 * hl + 2))
                            else:
                                mm(ps[pX][:, hl * 256 + 128:(hl + 1) * 256], MT[cur][:, hl, :], MP[cur][:, hl, 128:256],
                                   True, True, MT_s[cur] + MP_s[cur], psq(pX, 2 * hl + 1, 2 * hl + 2))
                            mm(ps[5][:, hl * 128:(hl + 1) * 128], MP[cur][:, hl, 0:128], MT[cur][:, hl, :], True, True,
                               MT_s[cur] + MP_s[cur], psq(5, hl, hl + 1))
                        vX = ps[pX][:, :].rearrange("p (a b) -> p a b", a=2)
                        if j < 5:
                            cp("act", MP[nxt][:, :, 0:128], vX[:, :, 0:128], psq(pX), MP_s[nxt])
                        tt("dve", MP[nxt][:, :, 128:256], vX[:, :, 128:256], MP[cur][:, :, 128:256], ALU.add,
                           psq(pX) + MP_s[cur], MP_s[nxt])
                        cp("act", MT[nxt][:], ps[5][:, 0:256].rearrange("p (a b) -> p a b", a=2), psq(5, 0, 2),
                           MT_s[nxt])
                        cur = nxt
                    for hl in range(2):
                        mm(ps[3][:, hl * 256 + 128:(hl + 1) * 256], MT[cur][:, hl, :], MP[cur][:, hl, 128:256], True, True,
                           MT_s[cur] + MP_s[cur], psq(3, 2 * hl + 1, 2 * hl + 2))
                    tt("dve", TT_[:], ps[3][:, :].rearrange("p (a b) -> p a b", a=2)[:, :, 128:256],
                       MP[cur][:, :, 128:256], ALU.add, psq(3) + MP_s[cur], TT_s)
                    for hl in range(2):
                        r_ = rows2[hl]
                        hs_ = slice(hl * 64, hl * 64 + 64)
                        mm(ps[6][:, hs_], AR[r_, ch, 0:128], STt[r_, g, :], True, False, AR_s + [ST_s[g]],
                           psq(6, 0, 1))
                        mm(ps[6][:, hs_], E2[:, hl, 0:128], TM[:, 0, hs_], False, True, E2_s + TM_s, psq(6, 0, 1))
                    cp("act", XS[:], ps[6][:, 0:128], psq(6, 0, 1), XS_s)
                    for hl in range(2):
                        hs_ = slice(hl * 64, hl * 64 + 64)
                        mm(ps[6][:, 128 + hl * 64:128 + hl * 64 + 64], TT_[:, hl, :], XS[:, hs_], True, True,
                           TT_s + XS_s, psq(6, 1, 2))
                    cp("act", UU[:], ps[6][:, 128:256], psq(6, 1, 2), UU_s)
                    for _it in range(NREF):
                        for hl in range(2):
                            hs_ = slice(hl * 64, hl * 64 + 64)
                            mm(ps[6][:, 128 + hl * 64:128 + hl * 64 + 64], NC[:, hl, :], UU[:, hs_], True, True,
                               NC_s + UU_s, psq(6))
                        tt("dve", YQ[:], ps[6][:, 128:256], XS[:], ALU.add, psq(6) + XS_s, YQ_s)
                        tt("pool", YQ[:], YQ[:], UU[:], ALU.subtract, YQ_s + UU_s, YQ_s)
                        for hl in range(2):
                            hs_ = slice(hl * 64, hl * 64 + 64)
                            mm(ps[6][:, 128 + hl * 64:128 + hl * 64 + 64], TT_[:, hl, :], YQ[:, hs_], True, True,
                               TT_s + YQ_s, psq(6))
                        tt("dve", UU[:], ps[6][:, 128:256], UU[:], ALU.add, psq(6) + UU_s, UU_s)
                    for hl in range(2):
                        r_ = rows2[hl]
                        hs_ = slice(hl * 64, hl * 64 + 64)
                        o_ = ps[6][:, 256 + hl * 64:256 + hl * 64 + 64]
                        mm(o_, AR[r_, ch, 128:256], STt[r_, g, :], True, False, AR_s + [ST_s[g]], psq(6, 2, 3))
                        mm(o_, ARB[:, hl, :], UU[:, hs_], False, False, ARB_s + UU_s, psq(6, 2, 3))
                        mm(o_, E2[:, hl, 128:256], TM[:, 0, hs_], False, True, E2_s + TM_s, psq(6, 2, 3))
                    cp("act", YT[:], ps[6][:, 256:384], psq(6, 2, 3), YT_s)
                    mm(ps[6][:, 384:512], TM[:, 1, :], UU[:], True, False, TM_s + UU_s, psq(6, 3, 4))
                    mm(ps[6][:, 384:512], TM[:, 2, :], TM[:, 0, :], False, True, TM_s, psq(6, 3, 4))
                    for hl in range(2):
                        r_ = rows2[hl]
                        stt("dve", STt[r_, g, :], STt[r_, g, :], Wt["eL"][r_, ch * 128 + 127: ch * 128 + 128],
                            ps[6][r_, 384 + hl * 64:384 + hl * 64 + 64], ALU.mult, ALU.add,
                            [ST_s[g]] + Ws["eL"] + psq(6, 3, 4), [ST_s[g]])
                    y3 = YT[:].rearrange("p (a b) -> p a b", a=2)
                    P.add("dve", lambda e, y3=y3: e.tensor_reduce(out=st4[:, 0:2], in_=y3, axis=AX.X, op=ALU.add),
                          YT_s, st4_s)
                    tt("pool", YQ[:], YT[:], YT[:], ALU.mult, YT_s, YQ_s)
                    q3 = YQ[:].rearrange("p (a b) -> p a b", a=2)
                    P.add("dve", lambda e, q3=q3: e.tensor_reduce(out=st4[:, 2:4], in_=q3, axis=AX.X, op=ALU.add),
                          YQ_s, st4_s)
                    ts("dve", st4[:, 0:4], st4[:, 0:4], 1.0 / 64, ALU.mult, st4_s, st4_s)
                    tt("dve", st4[:, 4:6], st4[:, 0:2], st4[:, 0:2], ALU.mult, st4_s, st4_s)
                    tt("dve", st4[:, 2:4], st4[:, 2:4], st4[:, 4:6], ALU.subtract, st4_s, st4_s)
                    act(st4[:, 2:4], st4[:, 2:4], AF.Sqrt, st4_s + [cols_s], st4_s, bias=col(co["gn_eps"]))
                    recip(st4[:, 2:4], st4[:, 2:4], st4_s, st4_s)
                    for hl in range(2):
                        hs_ = slice(hl * 64, hl * 64 + 64)
                        ts("dve", YQ[:, hs_], YT[:, hs_], st4[:, hl:hl + 1], ALU.subtract, YT_s + st4_s, YQ_s,
                           s2=st4[:, 2 + hl:3 + hl], op1=ALU.mult)
                    tr(ps[2][:, 384:512], YQ[:], id1, YQ_s + [ident_s], psq(2, 3, 4))
                    ts("dve", Wt["YF"][:, cs], ps[2][:, 384:512], col(co["rwkv_ln_w"] + cg_), ALU.mult,
                       psq(2, 3, 4) + [cols_s], Ws["YF"], s2=col(co["rwkv_ln_b"] + cg_), op1=ALU.add)
                tt("pool", Wt["YF"][:], Wt["YF"][:], Wt["BON"][:], ALU.add, Ws["YF"] + Ws["BON"], Ws["YF"])
                tt("pool", YA[:, g, :], Wt["YF"][:], Wt["G"][:], ALU.mult, Ws["YF"] + Ws["G"], [YA_s[g]])
            dma("sp", oT_d[0:512, tI * T:(tI + 1) * T].rearrange("(c p) t -> p c t", p=128), YA[:], YA_s,
                [oT_s[0][tI]], "do0")

    def hyb_b(layer):
        i = layer // 2
        co = col_off
        rows2 = [slice(0, 64), slice(64, 128)]
        reset()
        wB, wB_s = load_w(W["hyb_w_in"][i][:, A_COLS:HYB_COLS], KC, 2048)
        wO, wO_s = load_w(W["hyb_w_out"][i], KC, D)
        cosT, cos_s = alloc([NCH, 32], F32)
        sinT, sin_s = alloc([NCH, 32], F32)
        dmT, dm_s = alloc([8, 128], F32)
        zt, zt_s = alloc([8, 64], F32)
        xi, xi_s = alloc([4, 128], F32)
        cd, cd_s = alloc([4, 128], F32)
        bm, bm_s = alloc([4, 128], F32)
        dma("sp", bm[:], CT["bm4"], [], bm_s, "kb6")
        dma("sp", cosT[:], CT["cos"], [], cos_s, "kb0")
        dma("sp", sinT[:], CT["sin"], [], sin_s, "kb1")
        dma("sp", dmT[:], CT["dmatT"], [], dm_s, "kb2")
        dma("sp", zt[:], CT["zeta"], [], zt_s, "kb3")
        dma("sp", xi[:], CT["xi"], [], xi_s, "kb4")
        dma("sp", cd[:], CT["cd"], [], cd_s, "kb5")
        hb, hb_s = alloc([KC, T], F32, KC)
        hn, hn_s = alloc([KC, T], BF16, KC)
        SG, SG_s = alloc([4, T], F32, 4)
        YALL, YALL_s = alloc([8, T], BF16, 8)
        XQ, XQ_s = alloc([8, 64], F32)
        XK, XK_s = alloc([8, 64], F32)
        tA, tA_s = alloc([8, 32], F32)
        tB, tB_s = alloc([8, 32], F32)
        QR, QR_s = alloc([8, 64], F32)
        KR, KR_s = alloc([8, 64], F32)
        KZ, KZ_s = alloc([8, 64], BF16)
        VT, VT_s = alloc([512], BF16)
        QT, QT_s = alloc([4, 128], BF16)
        KTt, KT_s = alloc([4, 128], BF16)
        QX, QX_s = alloc([4, 128], BF16)
        SC, SC_s = alloc([8, 128], BF16, 2)
        OS, OS_s = alloc([8, 64], F32)
        OQ, OQ_s = alloc([8, 64], F32)
        ON, ON_s = alloc([8, 64], F32, 8)
        s8, s8_s = alloc([8], F32)
        Rt, R_s = alloc([4, 128], F32)
        Rb, Rb_s = alloc([4, 128], BF16)
        SCF, SCF_s = alloc([8, 128], F32, 2)
        KV, KV_s = alloc([4, 128], F32)
        KVm, KVm_s = alloc([4, 128], F32)
        ONT, ONT_s = alloc([512], F32)
        P.add("pool", lambda e: e.memset(Rt[:], 0.0), [], R_s)
        P.add("pool", lambda e: e.memset(Rb[:], 0.0), [], Rb_s)
        psb3 = ps[3].bitcast(BF16)
        psb7 = ps[7].bitcast(BF16)
        for tI in range(NT):
            load_h(tI, hb, hb_s, "hl0")
            dma("sp", hn[:], ft(hnT, tI), [hnT_s[tI]], hn_s, "hnl0")
            if "noa" in parts:
                P.add("pool", lambda e: e.memset(YALL[:, 0:4, :], 0.0), [], YALL_s[0:4])
            else:
                dma("sp", YALL[:, 0:4, :], oT_d[0:512, tI * T:(tI + 1) * T].rearrange("(c p) t -> p c t", p=128),
                    [oT_s[0][tI]], YALL_s[0:4], "do0")
            if "nob" in parts or CUT < 9:
                P.add("pool", lambda e: e.memset(YALL[:, 4:8, :], 0.0), [], YALL_s[4:8])
            for c in range(4 if "nob" not in parts else 0):
                pb = c % 2
                for k in range(KC):
                    mm(ps[pb][:, :], wB[:, k, 1536 + c * 128: 1536 + (c + 1) * 128], hn[:, k, :], k == 0, k == KC - 1,
                       wB_s + [hn_s[k]], psq(pb))
                act(SG[:, c, :], ps[pb][:, :], AF.Silu, psq(pb), [SG_s[c]])
            for ch in range(4 if "nob" not in parts else 0):
                cs = slice(ch * 128, (ch + 1) * 128)
                cg = tI * 4 + ch
                for sec in range(3):
                    for k in range(KC):
                        mm(ps[sec][:, :], hn[:, k, cs], wB[:, k, sec * 512:(sec + 1) * 512], k == 0, k == KC - 1,
                           wB_s + [hn_s[k]], psq(sec))
                cp("act", XQ[:].rearrange("p a b -> p (a b)"), ps[0][:, :], psq(0), XQ_s)
                cp("act", XK[:].rearrange("p a b -> p (a b)"), ps[1][:, :], psq(1), XK_s)
                cp("act", VT[:], ps[2][:, :], psq(2), VT_s)
                if CUT < 2:
                    continue
                cb = cosT[:, cg, :].unsqueeze(1).to_broadcast([128, 8, 32])
                sbc = sinT[:, cg, :].unsqueeze(1).to_broadcast([128, 8, 32])
                for (X, X_s, O, O_s) in ((XQ, XQ_s, QR, QR_s), (XK, XK_s, KR, KR_s)):
                    tt("dve", tA[:], X[:, :, 0:32], cb, ALU.mult, X_s + cos_s, tA_s)
                    tt("pool", tB[:], X[:, :, 32:64], sbc, ALU.mult, X_s + sin_s, tB_s)
                    tt("dve", O[:, :, 0:32], tA[:], tB[:], ALU.subtract, tA_s + tB_s, O_s)
                    tt("pool", tA[:], X[:, :, 0:32], sbc, ALU.mult, X_s + sin_s + O_s, tA_s)
                    tt("dve", tB[:], X[:, :, 32:64], cb, ALU.mult, X_s + cos_s + O_s, tB_s)
                    tt("pool", O[:, :, 32:64], tA[:], tB[:], ALU.add, tA_s + tB_s, O_s)
                if CUT < 3:
                    continue
                if os.environ.get("SKIPKZ") is None:
                    tt("pool", KZ[:], KR[:], zt[:], ALU.mult, KR_s + zt_s, KZ_s)
                if CUT < 4:
                    continue
                QRf = QR[:].rearrange("p a b -> p (a b)")
                KRf = KR[:].rearrange("p a b -> p (a b)")
                KZf = KZ[:].rearrange("p a b -> p (a b)")
                for p in range(4):
                    tr(ps[3][:, p * 128:(p + 1) * 128], QRf[:, p * 128:(p + 1) * 128], id1, QR_s + [ident_s],
                       psq(3, p, p + 1))
                    tr(ps[5][:, p * 128:(p + 1) * 128], KRf[:, p * 128:(p + 1) * 128], id1,
                       KR_s + [ident_s], psq(5, p, p + 1))
                STG = int(os.environ.get("STG4", "9"))
                if STG >= 2:
                    cp("act", QT[:].rearrange("p a b -> p (a b)"), ps[3][:, :], psq(3), QT_s)
                if STG >= 3:
                    tt("pool", QX[:], QT[:], xi[:], ALU.mult, QT_s + xi_s, QX_s)
                if STG >= 4:
                    cp("dve", KTt[:].rearrange("p a b -> p (a b)"), ps[5][:, :], psq(5), KT_s)
                if CUT < 5:
                    continue
                for h in range(8):
                    p = h // 2
                    r_ = rows2[h % 2]
                    pb = 4 + h % 2
                    mm(ps[pb][:, (h // 2) * 128:(h // 2 + 1) * 128], KTt[r_, p, :], QT[r_, p, :], True, True,
                       KT_s + QT_s, psq(pb))
                for hh in range(2):
                    cp("act", SCF[:, hh * 4:(hh + 1) * 4, :].rearrange("p a b -> p (a b)"), ps[4 + hh][:, :], psq(4 + hh),
                       [SCF_s[hh]])
                    tt("pool", SC[:, hh * 4:(hh + 1) * 4, :], SCF[:, hh * 4:(hh + 1) * 4, :],
                       dmT[:, hh * 4:(hh + 1) * 4, :], ALU.mult, [SCF_s[hh]] + dm_s, [SC_s[hh]])
                if CUT < 6:
                    continue
                for p in range(4):
                    mm(ps[6][:, p * 128:(p + 1) * 128], QX[:, p, :], Rb[:, p, :], True, False, QX_s + Rb_s, psq(6))
                    for hl in range(2):
                        h = 2 * p + hl
                        hs_ = slice(h * 64, (h + 1) * 64)
                        mm(ps[6][:, hs_], SC[:, (h % 2) * 4 + h // 2, :], VT[:, hs_], False, hl == 1,
                           [SC_s[h % 2]] + VT_s, psq(6))
                if CUT < 7:
                    continue
                cp("act", OS[:].rearrange("p a b -> p (a b)"), ps[6][:, :], psq(6), OS_s)
                tt("pool", OQ[:], OS[:], OS[:], ALU.mult, OS_s, OQ_s)
                P.add("dve", lambda e: e.tensor_reduce(out=s8[:], in_=OQ[:], axis=AX.X, op=ALU.add), OQ_s, s8_s)
                act(s8[:], s8[:], AF.Sqrt, s8_s + [cols_s], s8_s, bias=col(co["eps"]), scale=1.0 / 64)
                recip(s8[:], s8[:], s8_s, s8_s)
                for h in range(8):
                    stt("dve", ON[:, h, :], OS[:, h, :], s8[:, h:h + 1], onesf[:, 0:64], ALU.mult, ALU.mult,
                        OS_s + s8_s + [onesf_s], [ON_s[h]])
                if CUT < 8:
                    continue
                ONf = ON[:].rearrange("p a b -> p (a b)")
                for p in range(4):
                    tr(ps[7][:, p * 128:(p + 1) * 128], ONf[:, p * 128:(p + 1) * 128], id1, ON_s + [ident_s],
                       psq(7, p, p + 1))
                cp("act", ONT[:], ps[7][:, :], psq(7), ONT_s)
                for p in range(4):
                    stt("dve", YALL[:, 4 + p, cs], ONT[:, p * 128:(p + 1) * 128], col(co["ret_ln_w"] + i * 4 + p),
                        SG[:, p, cs], ALU.mult, ALU.mult, ONT_s + [cols_s, SG_s[p]], [YALL_s[4 + p]])
                if CUT < 9:
                    continue
                for p in range(4):
                    mm(ps[2][:, p * 128:(p + 1) * 128], KZf[:, p * 128:(p + 1) * 128], VT[:, p * 128:(p + 1) * 128],
                       True, True, KZ_s + VT_s, psq(2, p, p + 1))
                cp("act", KV[:].rearrange("p a b -> p (a b)"), ps[2][:, :], psq(2), KV_s)
                tt("pool", KVm[:], KV[:], bm[:], ALU.mult, KV_s + bm_s, KVm_s)
                tt("pool", Rt[:], Rt[:], cd[:], ALU.mult, R_s + cd_s, R_s)
                tt("pool", Rt[:], Rt[:], KVm[:], ALU.add, R_s + KVm_s, R_s)
                cp("pool", Rb[:], Rt[:], R_s, Rb_s)
            out_proj_add(wO, wO_s, YALL, YALL_s, KC, hb, hb_s)
            store_h(tI, hb, hb_s, "hs0")

    phase_input()
    if "xattn" in parts:
        phase_mem()
    for layer in layers:
        if "mix" in parts:
            if layer % 2 == 0:
                phase_hyb(layer)
            else:
                phase_diff(layer)
        if "xattn" in parts:
            phase_xattn(layer)
        if "mlp" in parts:
            phase_mlp(layer, 0)
            phase_mlp(layer, 1)
    phase_final()

    with nc.Block() as block:
        P.emit(nc, st, block)
    st.close()
    return nc, P


def make_cols(inp):
    names = []
    vecs = []

    def addv(nm, v):
        names.append(nm); vecs.append(v)

    addv("eps", np.full(128, EPS, np.float32))
    addv("gn_eps", np.full(128, GN_EPS, np.float32))
    addv("mem_norm_w", inp["mem_norm_w"])
    addv("final_norm_w", inp["final_norm_w"])
    addv("norm_mix_w", inp["norm_mix_w"].reshape(-1))
    addv("norm_cross_w", inp["norm_cross_w"].reshape(-1))
    addv("norm_mlp_w", inp["norm_mlp_w"].reshape(-1))
    addv("rwkv_mu", inp["rwkv_mu"].reshape(-1))
    for nm in ("rwkv_w0", "rwkv_a0", "rwkv_k_k", "rwkv_k_a", "rwkv_ln_w", "rwkv_ln_b", "ret_ln_w"):
        addv(nm, inp[nm].reshape(-1))
    addv("rwkv_r_k", inp["rwkv_r_k"].reshape(-1))
    addv("diff_ln_w", inp["diff_ln_w"].reshape(-1))
    arr, offs = pack_cols(vecs)
    return arr, dict(zip(names, offs))


def host_inputs(inp, S):
    cols, col_off = make_cols(inp)
    shared = {}
    for nm in ("xattn_w_q", "xattn_w_kv", "xattn_w_o", "mlp_w1", "mlp_w2", "hyb_w_in", "hyb_w_out",
               "diff_w_in", "diff_w_out", "rwkv_w2", "rwkv_a2", "rwkv_g2"):
        shared[nm] = np.ascontiguousarray(inp[nm], dtype=np.float32)
    shared["cols"] = cols
    lr = np.concatenate([inp["diff_lq1"], inp["diff_lk1"], inp["diff_lq2"], inp["diff_lk2"]], axis=1)
    shared["lrows"] = np.ascontiguousarray(np.broadcast_to(lr[:, None, :], (2, 128, 256)), dtype=np.float32)
    i = np.arange(128)
    rel0 = i[None, :] - i[:, None]
    b0 = inp["rel_bias"][t5_bucket_np(rel0)]
    b0 = np.where((rel0 >= 0)[:, :, None], b0, np.float32(NEG))
    b1 = inp["rel_bias"][t5_bucket_np(rel0 + 128)]
    bt = np.stack([b0, b1], axis=1).transpose(0, 1, 3, 2)
    shared["biasT"] = np.ascontiguousarray(bt, dtype=np.float32)
    shared["cfar"] = np.ascontiguousarray(np.broadcast_to(inp["rel_bias"][31][None, :], (128, 8)), dtype=np.float32)
    for nm, arr in host_consts(S).items():
        shared["c_" + nm] = arr
    return shared, cols.shape[1], col_off


_CACHE = {}


def run(inp, S, layers, parts, n_cores):
    shared, ncols, col_off = host_inputs(inp, S)
    key = (S, tuple(layers), tuple(parts))
    nc, P = build(S, layers, parts, ncols=ncols, col_off=col_off)
    in_maps = []
    for c in range(n_cores):
        m = dict(shared)
        m["x"] = np.ascontiguousarray(inp["x"][c, :S], dtype=np.float32)
        m["mem"] = np.ascontiguousarray(inp["mem"][c], dtype=np.float32)
        in_maps.append(m)
    res = run_bass_kernel_spmd(nc, in_maps, core_ids=list(range(n_cores)))
    return np.stack([r["out"] for r in res.results], axis=0)


def kernel(**inputs):
    inp = {k: np.asarray(v) for k, v in inputs.items()}
    out = run(inp, 4096, [0, 1, 2, 3], ("mix", "xattn", "mlp"), 8)
    return out.astype(np.float32)
```
